# Optimizing a Trainium2 kernel written in Bass

```python
import jax, jax.numpy as jnp
from jax import lax
import numpy as np

D_MODEL = 1024
BATCH = 8
SEQ = 4096
DEPTH = 4

N_MIXERS = 2
EPS = 1e-6

GLA_HEADS = 4
GLA_DK = D_MODEL // 2
GLA_DV = D_MODEL
GLA_HEAD_DK = GLA_DK // GLA_HEADS
GLA_HEAD_DV = GLA_DV // GLA_HEADS
GLA_GATE_RANK = 16
GLA_GATE_TAU = 16.0
GLA_CHUNK = 64
GLA_IN = 2 * GLA_DK + 2 * GLA_DV + GLA_GATE_RANK

CONV_E = D_MODEL
CONV_K = 31
CONV_IN = 3 * CONV_E

N_GLA_LAYERS = (DEPTH + 1) // 2
N_CONV_LAYERS = DEPTH // 2

kernel_name = 'hybrid_gla_conformer_gated'


def rmsnorm(x, w):
    x32 = x.astype(jnp.float32)
    y = x32 * lax.rsqrt(jnp.mean(x32 * x32, axis=-1, keepdims=True) + EPS)
    return (y * w.astype(jnp.float32)).astype(x.dtype)


def layernorm(x, w, b):
    x32 = x.astype(jnp.float32)
    mu = jnp.mean(x32, axis=-1, keepdims=True)
    xc = x32 - mu
    var = jnp.mean(xc * xc, axis=-1, keepdims=True)
    y = xc * lax.rsqrt(var + EPS)
    return (y * w.astype(jnp.float32) + b.astype(jnp.float32)).astype(x.dtype)


def gla_chunked(q, k, v, log_a):
    C = q.shape[3]
    b = jnp.cumsum(log_a, axis=3)
    b_last = b[:, :, :, -1:, :]
    q_dec = q * jnp.exp(b)
    k_inv = k * jnp.exp(-b)
    k_end = k * jnp.exp(b_last - b)
    causal = jnp.tril(jnp.ones((C, C), dtype=bool))
    scores = jnp.einsum('bhnid,bhnjd->bhnij', q_dec, k_inv)
    scores = jnp.where(causal, scores, 0.0)
    o_intra = jnp.einsum('bhnij,bhnjv->bhniv', scores, v)

    decay = jnp.exp(b_last[:, :, :, 0, :])
    xs = (jnp.moveaxis(q_dec, 2, 0), jnp.moveaxis(k_end, 2, 0),
          jnp.moveaxis(v, 2, 0), jnp.moveaxis(decay, 2, 0))
    B, H = q.shape[0], q.shape[1]
    s0 = jnp.zeros((B, H, q.shape[-1], v.shape[-1]), jnp.float32)

    def step(S, inp):
        qd, ke, vc, dc = inp
        o_inter = jnp.einsum('bhid,bhdv->bhiv', qd, S)
        S_new = dc[..., None] * S + jnp.einsum('bhjd,bhjv->bhdv', ke, vc)
        return S_new, o_inter

    _, o_inter = lax.scan(step, s0, xs)
    return o_intra + jnp.moveaxis(o_inter, 0, 2)


def gla_mixer(h, w_in, w_g2, b_g, gn_w, w_out):
    B, T, _ = h.shape
    N = T // GLA_CHUNK
    proj = h @ w_in
    q, k, v, z, g_lr = jnp.split(
        proj, [GLA_DK, 2 * GLA_DK, 2 * GLA_DK + GLA_DV, 2 * GLA_DK + 2 * GLA_DV], axis=-1)
    log_a = jax.nn.log_sigmoid((g_lr @ w_g2 + b_g).astype(jnp.float32)) / GLA_GATE_TAU

    def heads(t, d):
        return t.astype(jnp.float32).reshape(B, N, GLA_CHUNK, GLA_HEADS, d).transpose(0, 3, 1, 2, 4)

    qh = heads(q, GLA_HEAD_DK) * (GLA_HEAD_DK ** -0.5)
    kh = heads(k, GLA_HEAD_DK)
    vh = heads(v, GLA_HEAD_DV)
    ah = heads(log_a, GLA_HEAD_DK)
    o = gla_chunked(qh, kh, vh, ah)
    o = o.transpose(0, 2, 3, 1, 4).reshape(B, T, GLA_HEADS, GLA_HEAD_DV)
    o = o * lax.rsqrt(jnp.mean(o * o, axis=-1, keepdims=True) + EPS)
    o = o * gn_w.astype(jnp.float32).reshape(GLA_HEADS, GLA_HEAD_DV)
    o = o.reshape(B, T, GLA_DV).astype(h.dtype) * jax.nn.silu(z)
    return o @ w_out


def conv_mixer(h, w_in, dw_w, dw_b, ln_w, ln_b, w_out):
    a, gl, z = jnp.split(h @ w_in, 3, axis=-1)
    u = a * jax.nn.sigmoid(gl)
    u = lax.conv_general_dilated(
        u, dw_w.reshape(CONV_K, 1, CONV_E).astype(u.dtype),
        window_strides=(1,), padding=[(CONV_K - 1, 0)],
        dimension_numbers=('NWC', 'WIO', 'NWC'),
        feature_group_count=CONV_E) + dw_b
    u = jax.nn.silu(layernorm(u, ln_w, ln_b))
    return (u * jax.nn.silu(z)) @ w_out


def setup_inputs(seed: int = 0) -> dict:
    key = jax.random.key(seed)
    ks = jax.random.split(key, 16)
    f32 = jnp.float32
    nrm = lambda k, shape, s: jax.random.normal(k, shape, f32) * s
    return {
        'x': nrm(ks[0], (BATCH, SEQ, D_MODEL), 1.0),
        'norm_w': 1.0 + nrm(ks[1], (DEPTH, D_MODEL), 0.02),
        'final_norm_w': 1.0 + nrm(ks[2], (D_MODEL,), 0.02),
        'gla_w_in': nrm(ks[3], (N_GLA_LAYERS, D_MODEL, GLA_IN), D_MODEL ** -0.5),
        'gla_w_g2': nrm(ks[4], (N_GLA_LAYERS, GLA_GATE_RANK, GLA_DK), GLA_GATE_RANK ** -0.5),
        'gla_b_g': nrm(ks[5], (N_GLA_LAYERS, GLA_DK), 0.02),
        'gla_gn_w': 1.0 + nrm(ks[6], (N_GLA_LAYERS, GLA_DV), 0.02),
        'gla_w_out': nrm(ks[7], (N_GLA_LAYERS, GLA_DV, D_MODEL), GLA_DV ** -0.5),
        'conv_w_in': nrm(ks[8], (N_CONV_LAYERS, D_MODEL, CONV_IN), D_MODEL ** -0.5),
        'conv_dw_w': nrm(ks[9], (N_CONV_LAYERS, CONV_K, CONV_E), CONV_K ** -0.5),
        'conv_dw_b': nrm(ks[10], (N_CONV_LAYERS, CONV_E), 0.02),
        'conv_ln_w': 1.0 + nrm(ks[11], (N_CONV_LAYERS, CONV_E), 0.02),
        'conv_ln_b': nrm(ks[12], (N_CONV_LAYERS, CONV_E), 0.02),
        'conv_w_out': nrm(ks[13], (N_CONV_LAYERS, CONV_E, D_MODEL), CONV_E ** -0.5),
    }


def reference(x, norm_w, final_norm_w, gla_w_in, gla_w_g2, gla_b_g, gla_gn_w, gla_w_out,
              conv_w_in, conv_dw_w, conv_dw_b, conv_ln_w, conv_ln_b, conv_w_out):
    for i in range(DEPTH):
        h = rmsnorm(x, norm_w[i])
        j = i // N_MIXERS
        if i % N_MIXERS == 0:
            y = gla_mixer(h, gla_w_in[j], gla_w_g2[j], gla_b_g[j], gla_gn_w[j], gla_w_out[j])
        else:
            y = conv_mixer(h, conv_w_in[j], conv_dw_w[j], conv_dw_b[j],
                           conv_ln_w[j], conv_ln_b[j], conv_w_out[j])
        x = x + y
    return rmsnorm(x, final_norm_w)
```

```python
import numpy as np
from contextlib import ExitStack
import concourse.bass as bass
import concourse.mybir as mybir
from concourse.bass_utils import run_bass_kernel_spmd

dt = mybir.dt
AF = mybir.ActivationFunctionType
ALU = mybir.AluOpType
F32, BF16 = dt.float32, dt.bfloat16

D = 1024
KC = 8
SEQ_FULL = 4096
T = 1024
NSUB = T // 512
NCH = T // 128
EPS = 1e-6
DKH = 128
NSLOT = 6
UNITS_PER_LAYER = 16
ARENA = 212736
ATOM = 64
COMPUTE = ("pe", "act", "dve", "pool")
STREAMS = ("pe", "act", "dve", "pool", "sp")

PV_NW, PV_FNW, PV_GN, PV_DWB, PV_LNW, PV_LNB, PV_DW, PV_N = 0, 32, 40, 56, 72, 88, 104, 600


class Op:
    __slots__ = ("id", "stream", "emit", "is_dma", "key", "ordinal", "deps",
                 "waits", "signal", "sigval", "seq", "vc")


class Sched:
    def __init__(self, nc, arena_bytes):
        self.nc = nc
        self.ops = []
        self.nat_sb = (arena_bytes + ATOM - 1) // ATOM
        self.nat_ps = 16384 // ATOM
        n = self.nat_sb + self.nat_ps
        self.W = np.full(n, -1, np.int64)
        self.R = {s: np.full(n, -1, np.int64) for s in COMPUTE}
        self.RD = np.full(n, -1, np.int64)
        self.dram = {}
        self.key_count = {}
        self.key_last = {}
        self.seq = {s: 0 for s in STREAMS}

    def rng(self, ap):
        sp = str(ap.space)
        es = mybir.dt.size(ap.dtype)
        dims = ap.ap
        pstride = dims[0][0]
        off = ap.offset % pstride if pstride > 0 else ap.offset
        ext = 1
        for (st, cnt) in dims[1:]:
            ext += (cnt - 1) * abs(st)
        lo = off * es
        hi = (off + ext) * es
        a0 = lo // ATOM
        a1 = (hi + ATOM - 1) // ATOM
        if "PSUM" in sp:
            a0 += self.nat_sb
            a1 += self.nat_sb
        return a0, a1

    def op(self, stream, emit, reads=(), writes=(), dreads=(), dwrites=(), dma_key=None):
        o = Op()
        o.id = len(self.ops)
        o.stream = stream
        o.emit = emit
        o.is_dma = dma_key is not None
        o.key = dma_key
        o.signal = False
        o.sigval = 0
        o.ordinal = 0
        o.waits = []
        o.seq = self.seq[stream]
        self.seq[stream] += 1
        deps = set()
        rr = [self.rng(a) for a in reads]
        wr = [self.rng(a) for a in writes]
        for (a0, a1) in rr:
            deps.update(np.unique(self.W[a0:a1]).tolist())
        for (a0, a1) in wr:
            deps.update(np.unique(self.W[a0:a1]).tolist())
            for s in COMPUTE:
                deps.update(np.unique(self.R[s][a0:a1]).tolist())
            deps.update(np.unique(self.RD[a0:a1]).tolist())
        for name in dreads:
            d = self.dram.setdefault(name, {"W": -1, "R": {}, "RD": -1})
            deps.add(d["W"])
        for name in dwrites:
            d = self.dram.setdefault(name, {"W": -1, "R": {}, "RD": -1})
            deps.add(d["W"])
            deps.update(d["R"].values())
            deps.add(d["RD"])
        if o.is_dma:
            deps.add(self.key_last.get(dma_key, -1))
            self.key_count[dma_key] = self.key_count.get(dma_key, 0) + 1
            o.ordinal = self.key_count[dma_key]
            self.key_last[dma_key] = o.id
            for (a0, a1) in rr:
                deps.update(np.unique(self.RD[a0:a1]).tolist())
        deps.discard(-1)
        for (a0, a1) in rr:
            if o.is_dma:
                self.RD[a0:a1] = o.id
            else:
                self.R[stream][a0:a1] = o.id
        for (a0, a1) in wr:
            self.W[a0:a1] = o.id
            for s in COMPUTE:
                self.R[s][a0:a1] = -1
            self.RD[a0:a1] = -1
        for name in dreads:
            d = self.dram[name]
            if o.is_dma:
                d["RD"] = o.id
            else:
                d["R"][stream] = o.id
        for name in dwrites:
            d = self.dram[name]
            d["W"] = o.id
            d["R"] = {}
            d["RD"] = -1
        o.deps = deps
        self.ops.append(o)
        return o

    def resolve(self):
        ops = self.ops
        vc = {s: {} for s in STREAMS}
        for o in ops:
            cur = vc[o.stream]
            need = {}
            for did in o.deps:
                d = ops[did]
                if d.is_dma:
                    sem = ("k", d.key)
                    val = d.ordinal
                else:
                    if d.stream == "pe" and o.stream == "pe":
                        continue
                    sem = ("s", d.stream)
                    val = d.seq + 1
                if val > need.get(sem, (0, None))[0]:
                    need[sem] = (val, d)
            for sem, (val, d) in need.items():
                if cur.get(sem, 0) >= val:
                    continue
                o.waits.append((sem, d))
                d.signal = True
                for k2, v2 in d.vc.items():
                    if cur.get(k2, 0) < v2:
                        cur[k2] = v2
                if cur.get(sem, 0) < val:
                    cur[sem] = val
            o.vc = dict(cur)
            if o.is_dma:
                k = ("k", o.key)
                o.vc[k] = max(o.vc.get(k, 0), o.ordinal)
            else:
                k = ("s", o.stream)
                o.vc[k] = max(o.vc.get(k, 0), o.seq + 1)
        cnt = {s: 0 for s in COMPUTE}
        for o in ops:
            if not o.is_dma and o.signal:
                cnt[o.stream] += 1
                o.sigval = cnt[o.stream]
        return cnt

    def emit_all(self, final_keys=()):
        nc = self.nc
        self.resolve()
        with ExitStack() as es:
            sems = {}
            for s in COMPUTE:
                sems[("s", s)] = es.enter_context(nc.semaphore("s_" + s))
            for i, k in enumerate(self.key_count):
                sems[("k", k)] = es.enter_context(nc.semaphore("k%d" % i))
            block = es.enter_context(nc.Block())
            by_stream = {s: [o for o in self.ops if o.stream == s] for s in STREAMS}

            def run(eng, stream):
                for o in by_stream[stream]:
                    for (sem, d) in o.waits:
                        if d.is_dma:
                            eng.wait_ge(sems[sem], 16 * d.ordinal)
                        else:
                            eng.wait_ge(sems[sem], d.sigval)
                    ins = o.emit(eng)
                    if o.is_dma:
                        ins.then_inc(sems[("k", o.key)], 16)
                    elif o.signal:
                        ins.then_inc(sems[("s", o.stream)], 1)
                if stream == "sp":
                    for k in final_keys:
                        eng.wait_ge(sems[("k", k)], 16 * self.key_count[k])

            @block.tensor
            def _(e):
                run(e, "pe")

            @block.scalar
            def _(e):
                run(e, "act")

            @block.vector
            def _(e):
                run(e, "dve")

            @block.gpsimd
            def _(e):
                run(e, "pool")

            @block.sync
            def _(e):
                run(e, "sp")


def build_program(seq=SEQ_FULL, nlayers=4):
    ntiles = seq // T
    nc = bass.Bass("TRN2", target_bir_lowering=False)
    xTd = nc.dram_tensor("xT", [D, seq], F32, kind="ExternalInput").ap()
    wts = nc.dram_tensor("wts", [4 * UNITS_PER_LAYER * 128, 2048], F32, kind="ExternalInput").ap()
    wgd = nc.dram_tensor("wg", [128, 2 * KC * 16], F32, kind="ExternalInput").ap()
    wg2d = nc.dram_tensor("wg2a", [17, 2 * 512], F32, kind="ExternalInput").ap()
    pvd = nc.dram_tensor("pv", [128, PV_N], F32, kind="ExternalInput").ap()
    outd = nc.dram_tensor("outT", [D, seq], F32, kind="ExternalOutput").ap()
    wbf = nc.dram_tensor("wbf", [4 * UNITS_PER_LAYER * 128, 2048], BF16, kind="Internal").ap()

    A = nc.alloc_sbuf_tensor("arena", [128, ARENA], dt.uint8)
    P = nc.alloc_psum_tensor("ps", [128, 4096], F32)
    S = Sched(nc, ARENA)
    off = [0]

    def sb(shape, d, at=None):
        nb = int(np.prod(shape[1:])) * mybir.dt.size(d)
        n = (nb + 63) // 64 * 64
        if at is None:
            o = off[0]
            off[0] += n
        else:
            o = at
        assert o + n <= ARENA, (o, n)
        v = A[:, o:o + nb].bitcast(d)
        if len(shape) == 3:
            v = v.rearrange("p (a b) -> p a b", a=shape[1])
        elif len(shape) == 4:
            v = v.rearrange("p (a b c) -> p a b c", a=shape[1], b=shape[2])
        return v

    xT = sb([128, KC, T], F32)
    hT = sb([128, KC, T], BF16)
    og = sb([128, KC, T], BF16)
    ring = [sb([128, KC, 256], BF16) for _ in range(NSLOT)]
    TMP = [sb([128, 512], F32) for _ in range(8)]
    TMPB = [sb([128, 512], BF16) for _ in range(4)]
    STAT = [sb([128, 512], F32) for _ in range(4)]
    ones_bf = sb([128, 128], BF16)
    ident = sb([128, 128], BF16)
    identB = sb([128, 32], BF16)
    utri = sb([128, 128], F32)
    mask = sb([128, 128], F32)
    ones_f = sb([128, 128], F32)
    cst = sb([128, 16], F32)
    pv = sb([128, PV_N], F32)
    dwh = sb([128, 496], F32)
    hln = sb([128, 32], F32)
    wg_bf = sb([128, 2 * KC * 16], BF16)
    wg2a = sb([128, 1024], F32)
    g_aug = sb([128, T], F32)
    Sst = sb([128, 8, 256], F32)
    Sall = sb([128, NCH, 256], BF16)
    stg = [sb([128, 1024], F32) for _ in range(2)]
    hist = sb([128, 16, 30], BF16)
    base = off[0]
    sp_ = sb([128, NCH, 512], F32)
    E1 = sb([128, T], F32)
    E2 = sb([128, T], F32)
    qdec = sb([128, T], BF16)
    kinv = sb([128, T], BF16)
    kendT = sb([128, T], BF16)
    kend = sb([128, NCH, 128], BF16)
    vtok = sb([128, NCH, 256], BF16)
    sz = sb([128, 2, T], BF16)
    oT = sb([128, 2, T], F32)
    osq = sb([128, 2, T], BF16)
    scm = sb([128, NCH, 128], BF16)
    gla_end = off[0]
    off[0] = base
    uc = sb([128, KC, T], F32)
    ubuf = [sb([128, 30 + T], BF16) for _ in range(2)]
    dgp = [sb([128, 31, 32], BF16) for _ in range(2)]
    acc1 = sb([128, T], F32)
    acc2 = sb([128, T], F32)
    conv_end = off[0]
    assert max(gla_end, conv_end) <= ARENA

    cnt = {"ps": 0, "tmp": 0, "tmpb": 0}

    def psum():
        i = cnt["ps"] % 7
        cnt["ps"] += 1
        return P[:, i * 512:(i + 1) * 512]

    ps_stat = P[:, 7 * 512:8 * 512]

    def tmp():
        i = cnt["tmp"] % len(TMP)
        cnt["tmp"] += 1
        return TMP[i], i

    def tmpb():
        i = cnt["tmpb"] % len(TMPB)
        cnt["tmpb"] += 1
        return TMPB[i]

    def isap(v):
        return not isinstance(v, (int, float)) and v is not None

    def mm(out, lhsT, rhs, start, stop):
        S.op("pe", lambda e: e.matmul(out, lhsT=lhsT, rhs=rhs, start=start, stop=stop),
             reads=[lhsT, rhs], writes=[out])

    def mmt(out, lhsT, rhs, start, stop, tp):
        S.op("pe", lambda e: e.matmul(out, lhsT=lhsT, rhs=rhs, start=start, stop=stop, tile_position=tp),
             reads=[lhsT, rhs], writes=[out])

    def tr(out, in_):
        S.op("pe", lambda e: e.transpose(out, in_, ident), reads=[in_, ident], writes=[out])

    def act(out, in_, func, scale=1.0, bias=0.0):
        rd = [in_] + [v for v in (scale, bias) if isap(v)]
        S.op("act", lambda e: e.activation(out=out, in_=in_, func=func, bias=bias, scale=scale),
             reads=rd, writes=[out])

    def tt(eng, out, in0, in1, op):
        S.op(eng, lambda e: e.tensor_tensor(out=out, in0=in0, in1=in1, op=op),
             reads=[in0, in1], writes=[out])

    def stt(out, in0, scalar, in1, op0, op1):
        rd = [in0, in1] + ([scalar] if isap(scalar) else [])
        S.op("dve", lambda e: e.scalar_tensor_tensor(out=out, in0=in0, scalar=scalar, in1=in1,
                                                     op0=op0, op1=op1),
             reads=rd, writes=[out])

    def ts(eng, out, in0, s1, s2, op0, op1=None):
        rd = [in0] + [v for v in (s1, s2) if isap(v)]
        if op1 is None:
            S.op(eng, lambda e: e.tensor_scalar(out=out, in0=in0, scalar1=s1, scalar2=None, op0=op0),
                 reads=rd, writes=[out])
        else:
            S.op(eng, lambda e: e.tensor_scalar(out=out, in0=in0, scalar1=s1, scalar2=s2,
                                                op0=op0, op1=op1),
                 reads=rd, writes=[out])

    def cp(eng, out, in_):
        if eng == "act":
            S.op("act", lambda e: e.activation(out=out, in_=in_, func=AF.Copy), reads=[in_], writes=[out])
        else:
            S.op(eng, lambda e: e.tensor_copy(out=out, in_=in_), reads=[in_], writes=[out])

    def memset(eng, ap, val):
        S.op(eng, lambda e: e.memset(ap, val), writes=[ap])

    def aselect(ap, cmp_op):
        S.op("pool", lambda e: e.affine_select(out=ap, in_=ap, pattern=[[1, 128]], compare_op=cmp_op,
                                               fill=0.0, base=0, channel_multiplier=-1),
             reads=[ap], writes=[ap])

    def dma(stream, out, in_, key, reads=(), writes=(), dreads=(), dwrites=()):
        S.op(stream, lambda e: e.dma_start(out=out, in_=in_), reads=reads, writes=writes,
             dreads=dreads, dwrites=dwrites, dma_key=key)

    eps_col = cst[:, 0:1]
    one_col = cst[:, 1:2]

    def pvc(col):
        return pv[:, col:col + 1]

    dma("pool", wg_bf, wgd, "wgc", writes=[wg_bf])
    dma("sp", pv, pvd, "pv", writes=[pv])
    memset("pool", wg2a, 0.0)
    dma("sp", wg2a[0:17, :], wg2d, "wg2", writes=[wg2a])
    memset("pool", ones_bf, 1.0)
    memset("pool", ones_f, 1.0)
    memset("dve", cst[:, 0:1], EPS)
    memset("dve", cst[:, 1:2], 1.0)
    memset("pool", ident, 1.0)
    aselect(ident, ALU.is_equal)
    tt("pool", identB, ident[:, 0:32], ident[:, 32:64], ALU.add)
    tt("pool", identB, identB, ident[:, 64:96], ALU.add)
    tt("pool", identB, identB, ident[:, 96:128], ALU.add)
    memset("pool", utri, -1.0 / 16.0)
    aselect(utri, ALU.is_ge)
    memset("pool", mask, 1.0)
    aselect(mask, ALU.is_ge)
    memset("dve", Sst, 0.0)
    memset("dve", hist, 0.0)
    memset("dve", g_aug, 1.0)
    ts("dve", dwh, pv[:, PV_DW:PV_DW + 496], 1.0, None, ALU.mult)
    ts("dve", hln, pv[:, PV_LNW:PV_LNW + 32], -1.0, None, ALU.mult)
    dwh4 = dwh.rearrange("p (a k) -> p a k", k=31)
    wg4 = wg_bf.rearrange("p (g c j) -> p g c j", g=2, c=KC)

    CONV_ORDER = [0, 1, 2, 8, 3, 4, 9, 5, 6, 10, 7, 11, 12, 13, 14, 15]
    GLA_ORDER = [1, 0, 2] + list(range(3, 16))
    units = [(ti, l, (GLA_ORDER[u] if l % 2 == 0 else CONV_ORDER[u])) for ti in range(ntiles) for l in range(nlayers)
             for u in range(UNITS_PER_LAYER)]
    rstate = {"next_load": 0, "next_use": 0}

    stg_i = [0]

    def ring_load():
        n = rstate["next_load"]
        if n >= len(units):
            return
        (ti, l, u) = units[n]
        slot = n % NSLOT
        row = (l * UNITS_PER_LAYER + u) * 128
        dst = ring[slot].rearrange("p a b -> p (a b)")
        if ti == 0:
            for half in range(2):
                k = stg_i[0] % 2
                stg_i[0] += 1
                dma("sp", stg[k], wts[row:row + 128, half * 1024:(half + 1) * 1024], ("stg", k),
                    writes=[stg[k]])
                cp("act" if half == 0 else "dve", dst[:, half * 1024:(half + 1) * 1024], stg[k])
        else:
            dma("sp", dst, wbf[row:row + 128, :], ("ring", slot), writes=[ring[slot]],
                dreads=[("wbf", l, u)])
        rstate["next_load"] += 1

    def ring_acquire(expect):
        n = rstate["next_use"]
        assert units[n][1:] == expect, (units[n], expect)
        return ring[n % NSLOT]

    def ring_acquire_n(expect, k):
        n = rstate["next_use"] + k
        assert units[n][1:] == expect, (units[n], expect)
        return ring[n % NSLOT]

    def ring_release():
        n = rstate["next_use"]
        (ti, l, u) = units[n]
        if ti == 0 and ntiles > 1:
            slot = n % NSLOT
            row = (l * UNITS_PER_LAYER + u) * 128
            dma("sp", wbf[row:row + 128, :], ring[slot].rearrange("p a b -> p (a b)"), ("wst", slot),
                reads=[ring[slot]], dwrites=[("wbf", l, u)])
        rstate["next_use"] += 1
        ring_load()

    for _ in range(NSLOT):
        ring_load()

    def ssl(s):
        return slice(s * 512, (s + 1) * 512)

    def rstd_from(ms, scale, lbuf, rbuf):
        act(lbuf, ms, AF.Ln, scale=scale, bias=eps_col)
        act(rbuf, lbuf, AF.Exp, scale=-0.5)
        return rbuf

    def sigmoid_multi(srcs, scales=None, biases=None):
        n = len(srcs)
        scales = scales or [-1.0] * n
        biases = biases or [0.0] * n
        outs = [tmp()[0] for _ in range(n)]
        for i in range(n):
            act(outs[i], srcs[i], AF.Exp, scale=scales[i], bias=biases[i])
        for i in range(n):
            act(outs[i], outs[i], AF.Ln, bias=one_col)
        for i in range(n):
            act(outs[i], outs[i], AF.Exp, scale=-1.0)
        return outs

    def rmsnorm_s(wcol0, out_fn, s, between=None):
        ms = ps_stat
        sqs = {}
        for c in range(min(2, KC)):
            sqs[c] = tmpb()
            act(sqs[c], xT[:, c, ssl(s)], AF.Square)
        for c in range(KC):
            if between is not None:
                between(c)
            mm(ms, ones_bf, sqs[c], c == 0, c == KC - 1)
            if c + 2 < KC:
                sqs[c + 2] = tmpb()
                act(sqs[c + 2], xT[:, c + 2, ssl(s)], AF.Square)
        r = rstd_from(ms, 1.0 / D, STAT[0], STAT[1])
        for c in range(KC):
            out_fn(c, s, r, pvc(wcol0 + c))

    def rmsnorm(wcol0, out_fn):
        for s in range(NSUB):
            rmsnorm_s(wcol0, out_fn, s)

    def proj_fm(U, col0, src, s, evac):
        ps = psum()
        for c in range(KC):
            mm(ps, U[:, c, col0:col0 + 128], src[:, c, ssl(s)], c == 0, c == KC - 1)
        evac(ps)

    def proj_ps(U, col0, src, s):
        ps = psum()
        for c in range(KC):
            mm(ps, U[:, c, col0:col0 + 128], src[:, c, ssl(s)], c == 0, c == KC - 1)
        return ps

    def outproj(l, nxt, during_s0=()):
        Us = []
        for j in range(4):
            Us.append(ring_acquire_n((l, 12 + j), j))

        def group(ob, s):
            j, m = ob // 2, ob % 2
            ps = proj_ps(Us[j], m * 128, og, s)
            tt("dve", xT[:, ob, ssl(s)], xT[:, ob, ssl(s)], ps, ALU.add)

        pend = list(during_s0)
        for ob in range(KC):
            group(ob, 0)
            if ob % 2 == 0 and pend:
                pend.pop(0)()
        for th in pend:
            th()

        def between(c):
            group(c, 1)
            if c % 2 == 1:
                ring_release()
        rmsnorm_s(nxt[0], nxt[1], 0, between=between)
        rmsnorm_s(nxt[0], nxt[1], 1)

    def hnorm_out(c, s, r, wc):
        stt(hT[:, c, ssl(s)], xT[:, c, ssl(s)], wc, r, ALU.mult, ALU.mult)

    def gla_layer(l, first, nxt):
        gi = l // 2
        if first:
            rmsnorm(PV_NW + l * 8, hnorm_out)
        for s in range(NSUB):
            gp = psum()[0:16, :]
            for c in range(KC):
                mm(gp, wg4[:, gi, c, :], hT[:, c, ssl(s)], c == 0, c == KC - 1)
            cp("act", g_aug[0:16, ssl(s)], gp)
        gla_vproj(l, 0)
        for j in range(NCH):
            xp = psum()
            mm(xp, g_aug[0:17, j * 128:(j + 1) * 128], wg2a[0:17, gi * 512:(gi + 1) * 512], True, True)
            e_, _ = tmp()
            act(e_, xp, AF.Exp, scale=-1.0)
            act(sp_[:, j, :], e_, AF.Ln, bias=one_col)
        for h in range(4):
            gla_head(l, gi, h)
        outproj(l, nxt)

    def gla_vproj(l, h):
        U = ring_acquire((l, 3 * h + 1))
        for jp in range(NCH // 2):
            vp = psum()
            for jj in range(2):
                j = jp * 2 + jj
                for c in range(KC):
                    mm(vp[:, jj * 256:(jj + 1) * 256], hT[:, c, j * 128:(j + 1) * 128], U[:, c, :],
                       c == 0, c == KC - 1)
            cp("act" if jp % 2 == 0 else "dve",
               vtok[:, jp * 2:jp * 2 + 2, :].rearrange("p a b -> p (a b)"), vp)
        ring_release()

    def gla_head(l, gi, h):
        Sh = Sst[:, gi * 4 + h, :]
        bt = [psum(), psum()]
        for j in range(NCH):
            mm(bt[j // 4][:, (j % 4) * 128:(j % 4 + 1) * 128], sp_[:, j, h * 128:(h + 1) * 128], utri,
               True, True)
        for s in range(NSUB):
            act(E1[:, ssl(s)], bt[s], AF.Exp)
            act(E2[:, ssl(s)], bt[s], AF.Exp, scale=-1.0)
        U = ring_acquire((l, 3 * h))
        for s in range(NSUB):
            def evq(ps, s=s):
                stt(qdec[:, ssl(s)], ps, DKH ** -0.5, E1[:, ssl(s)], ALU.mult, ALU.mult)
            proj_fm(U, 0, hT, s, evq)

            def evk(ps, s=s):
                tt("dve", kinv[:, ssl(s)], ps, E2[:, ssl(s)], ALU.mult)
                for jj in range(4):
                    j = s * 4 + jj
                    stt(kendT[:, j * 128:(j + 1) * 128], ps[:, jj * 128:(jj + 1) * 128],
                        E1[:, j * 128 + 127:j * 128 + 128], E2[:, j * 128:(j + 1) * 128],
                        ALU.mult, ALU.mult)
            proj_fm(U, 128, hT, s, evk)
        ring_release()
        if h != 0:
            gla_vproj(l, h)
        tp = psum().bitcast(BF16)
        for j in range(NCH):
            tr(tp[:, j * 128:(j + 1) * 128], kendT[:, j * 128:(j + 1) * 128])
        cp("act", kend.rearrange("p a b -> p (a b)"), tp)
        dsb = [psum() for _ in range(NCH // 2)]
        for j in range(NCH):
            mm(dsb[j // 2][:, (j % 2) * 256:(j % 2 + 1) * 256], kend[:, j, :], vtok[:, j, :], True, True)
        cp("dve", Sall[:, 0, :], Sh)
        for j in range(NCH):
            dcol = E1[:, j * 128 + 127:j * 128 + 128]
            dsj = dsb[j // 2][:, (j % 2) * 256:(j % 2 + 1) * 256]
            if j + 1 < NCH:
                stt(Sall[:, j + 1, :], Sh, dcol, dsj, ALU.mult, ALU.add)
            stt(Sh, Sh, dcol, dsj, ALU.mult, ALU.add)
        scb = [psum() for _ in range(NCH // 4)]
        for j in range(NCH):
            jsl = slice(j * 128, (j + 1) * 128)
            mm(scb[j // 4][:, (j % 4) * 128:(j % 4 + 1) * 128], kinv[:, jsl], qdec[:, jsl], True, True)
        for b in range(NCH // 4):
            tt("dve", scm[:, b * 4:(b + 1) * 4, :], scb[b].rearrange("p (a i) -> p a i", a=4),
               mask.unsqueeze(1).broadcast_to([128, 4, 128]), ALU.mult)
        U = ring_acquire((l, 3 * h + 2))
        for m in range(2):
            pss = [proj_ps(U, m * 128, hT, s) for s in range(NSUB)]
            gs = sigmoid_multi(pss)
            for s in range(NSUB):
                tt("dve", sz[:, m, ssl(s)], pss[s], gs[s], ALU.mult)
        ring_release()
        op_banks = []
        for jp in range(NCH // 2):
            opb = psum()
            op_banks.append(opb)
            for jj in range(2):
                j = jp * 2 + jj
                jsl = slice(j * 128, (j + 1) * 128)
                for m in range(2):
                    dst = opb[:, m * 256 + jj * 128:m * 256 + jj * 128 + 128]
                    mm(dst, vtok[:, j, m * 128:(m + 1) * 128], scm[:, j, :], True, False)
                    mm(dst, Sall[:, j, m * 128:(m + 1) * 128], qdec[:, jsl], False, True)
            pin = opb.rearrange("p (m i) -> p m i", m=2)
            cp("act", oT[:, :, jp * 256:(jp + 1) * 256], pin)
            tt("pool", osq[:, :, jp * 256:(jp + 1) * 256], oT[:, :, jp * 256:(jp + 1) * 256],
               oT[:, :, jp * 256:(jp + 1) * 256], ALU.mult)
        for s in range(NSUB):
            ms = psum()
            for m in range(2):
                mm(ms, ones_bf, osq[:, m, ssl(s)], m == 0, m == 1)
            r = rstd_from(ms, 1.0 / 256.0, STAT[0], STAT[1])
            for m in range(2):
                t1, _ = tmp()
                stt(t1, oT[:, m, ssl(s)], pvc(PV_GN + gi * 8 + h * 2 + m), r, ALU.mult, ALU.mult)
                tt("pool", og[:, h * 2 + m, ssl(s)], t1, sz[:, m, ssl(s)], ALU.mult)

    def conv_layer(l, first, nxt):
        ci = l // 2
        if first:
            rmsnorm(PV_NW + l * 8, hnorm_out)

        def conv_A(g):
            U = ring_acquire((l, g))
            ub = ubuf[g % 2]
            cp("pool", ub[:, 0:30], hist[:, ci * 8 + g, :])
            gps = [proj_ps(U, 128, hT, s) for s in range(NSUB)]
            gs = sigmoid_multi(gps)
            aps = [proj_ps(U, 0, hT, s) for s in range(NSUB)]
            for s in range(NSUB):
                tt("dve", ub[:, 30 + s * 512:30 + (s + 1) * 512], aps[s], gs[s], ALU.mult)
            ring_release()
            cp("pool", hist[:, ci * 8 + g, :], ub[:, T:T + 30])

        tapsets = [list(range(8 * j, min(8 * j + 8, 31))) for j in range(4)]

        def build_dg(g):
            idb = identB.unsqueeze(1).broadcast_to([128, 31, 32])
            wb = dwh4[:, ci * 8 + g, :].unsqueeze(2).broadcast_to([128, 31, 32])
            tt("dve", dgp[g % 2], idb, wb, ALU.mult)

        def conv_B(g):
            ub = ubuf[g % 2]
            dgg = dgp[g % 2]
            bcol = pvc(PV_DWB + ci * 8 + g)
            cpss = []
            for s in range(NSUB):
                bk = [psum() for _ in range(4)]
                for t in range(8):
                    for i in range(4):
                        for j in range(4):
                            if t >= len(tapsets[j]):
                                continue
                            k = tapsets[j][t]
                            mmt(bk[i][32 * j:32 * j + 32, :], dgg[32 * i:32 * i + 32, k, :],
                                ub[32 * i:32 * i + 32, s * 512 + k:s * 512 + k + 512],
                                t == 0, t == len(tapsets[j]) - 1, (32 * i, 32 * j))
                pis = []
                for i in range(4):
                    pi_ = tmpb()
                    cp("act" if i % 2 == 0 else "dve", pi_, bk[i])
                    pis.append(pi_)
                cps = psum()
                for i in range(4):
                    mmt(cps[32 * i:32 * i + 32, :], identB, pis[i], True, True, (0, 32 * i))
                cpss.append(cps)
            if g + 1 < KC:
                build_dg(g + 1)
            for s in range(NSUB):
                cps = cpss[s]
                act(uc[:, g, ssl(s)], cps, AF.Identity, bias=bcol)
                q_, _ = tmp()
                act(q_, cps, AF.Square, bias=bcol)
                if g == 0:
                    cp("pool", acc1[:, ssl(s)], uc[:, g, ssl(s)])
                    cp("pool", acc2[:, ssl(s)], q_)
                else:
                    tt("pool", acc1[:, ssl(s)], acc1[:, ssl(s)], uc[:, g, ssl(s)], ALU.add)
                    tt("pool", acc2[:, ssl(s)], acc2[:, ssl(s)], q_, ALU.add)

        def zblock(zb):
            zu, m = zb // 2, zb % 2
            U = ring_acquire((l, 8 + zu))
            pss = [proj_ps(U, m * 128, hT, s) for s in range(NSUB)]
            gs = sigmoid_multi(pss)
            for s in range(NSUB):
                tt("dve", og[:, zb, ssl(s)], pss[s], gs[s], ALU.mult)
            if m == 1:
                ring_release()

        build_dg(0)
        conv_A(0)
        for g in range(KC):
            if g + 1 < KC:
                conv_A(g + 1)
            conv_B(g)
            if g % 2 == 1:
                zblock(g - 1)
                zblock(g)

        mus, rs, mrs = [], [], []
        for s in range(NSUB):
            mp = psum()
            mm(mp, ones_f, acc1[:, ssl(s)], True, True)
            qp = psum()
            mm(qp, ones_f, acc2[:, ssl(s)], True, True)
            mu, r, mr = acc1[:, ssl(s)], acc2[:, ssl(s)], STAT[s]
            musq, var = STAT[2], STAT[3]
            ts("dve", mu, mp, 1.0 / D, None, ALU.mult)
            tt("dve", musq, mu, mu, ALU.mult)
            stt(var, qp, 1.0 / D, musq, ALU.mult, ALU.subtract)
            rstd_from(var, 1.0, musq, r)
            tt("dve", mr, mu, r, ALU.mult)
            mus.append(mu)
            rs.append(r)
            mrs.append(mr)

        def lnpair(s, g0):
            r, mr = rs[s], mrs[s]
            ys = []
            for g in (g0, g0 + 1):
                y_, _ = tmp()
                stt(y_, uc[:, g, ssl(s)], pvc(PV_LNW + ci * 8 + g), r, ALU.mult, ALU.mult)
                stt(y_, mr, hln[:, ci * 8 + g:ci * 8 + g + 1], y_, ALU.mult, ALU.add)
                ys.append(y_)
            gs = sigmoid_multi(ys, scales=[-1.0, -1.0],
                               biases=[hln[:, 16 + ci * 8 + g:16 + ci * 8 + g + 1] for g in (g0, g0 + 1)])
            for i, g in enumerate((g0, g0 + 1)):
                stt(ys[i], ys[i], pvc(PV_LNB + ci * 8 + g), gs[i], ALU.add, ALU.mult)
                tt("pool", og[:, g, ssl(s)], ys[i], og[:, g, ssl(s)], ALU.mult)

        order = [(0, 0), (0, 2), (0, 4), (0, 6)]
        for it in order:
            if isinstance(it, str):
                zblock(int(it[1:]))
            else:
                lnpair(it[0], it[1])
        outproj(l, nxt, during_s0=[lambda g0=g0: lnpair(1, g0) for g0 in (0, 2, 4, 6)])

    store_keys = []
    for ti in range(ntiles):
        t0 = ti * T
        for c in range(KC):
            dma("sp", xT[:, c, :], xTd[c * 128:(c + 1) * 128, t0:t0 + T], ("x", c), writes=[xT[:, c, :]])
        def fin_out(c, s, r, wc, t0=t0):
            o_, i = tmp()
            stt(o_, xT[:, c, ssl(s)], wc, r, ALU.mult, ALU.mult)
            key = ("st", i)
            if key not in store_keys:
                store_keys.append(key)
            dma("sp", outd[c * 128:(c + 1) * 128, t0 + s * 512:t0 + (s + 1) * 512], o_, key, reads=[o_])

        for l in range(nlayers):
            if l + 1 < nlayers:
                nxt = (PV_NW + (l + 1) * 8, hnorm_out)
            else:
                nxt = (PV_FNW, fin_out)
            if l % 2 == 0:
                gla_layer(l, l == 0, nxt)
            else:
                conv_layer(l, l == 0, nxt)

    S.emit_all(final_keys=store_keys)
    return nc, len(S.ops)


def _unit(wcols):
    return np.ascontiguousarray(wcols.reshape(KC, 128, 256).transpose(1, 0, 2).reshape(128, KC * 256))


def _pcol(v):
    return np.ascontiguousarray(np.asarray(v).reshape(KC, 128).T)


def prep_weights(inp):
    f = lambda k: np.asarray(inp[k], dtype=np.float32)
    gwi, gwo = f("gla_w_in"), f("gla_w_out")
    cwi, cwo = f("conv_w_in"), f("conv_w_out")
    units = []
    for l in range(4):
        j = l // 2
        if l % 2 == 0:
            W = gwi[j]
            for h in range(4):
                units.append(_unit(np.concatenate([W[:, h * 128:(h + 1) * 128],
                                                   W[:, 512 + h * 128:512 + (h + 1) * 128]], axis=1)))
                units.append(_unit(W[:, 1024 + h * 256:1024 + (h + 1) * 256]))
                units.append(_unit(W[:, 2048 + h * 256:2048 + (h + 1) * 256]))
            for o in range(4):
                units.append(_unit(gwo[j][:, o * 256:(o + 1) * 256]))
        else:
            W = cwi[j]
            for g in range(8):
                units.append(_unit(np.concatenate([W[:, g * 128:(g + 1) * 128],
                                                   W[:, 1024 + g * 128:1024 + (g + 1) * 128]], axis=1)))
            for o in range(4):
                units.append(_unit(W[:, 2048 + o * 256:2048 + (o + 1) * 256]))
            for o in range(4):
                units.append(_unit(cwo[j][:, o * 256:(o + 1) * 256]))
    wts = np.ascontiguousarray(np.concatenate(units, axis=0))
    wg = np.stack([gwi[gi][:, 3072:3088].reshape(KC, 128, 16).transpose(1, 0, 2) for gi in range(2)], axis=1)
    wg = np.ascontiguousarray(wg.reshape(128, 2 * KC * 16))
    wg2a = np.zeros((17, 1024), np.float32)
    for gi in range(2):
        wg2a[0:16, gi * 512:(gi + 1) * 512] = f("gla_w_g2")[gi]
        wg2a[16, gi * 512:(gi + 1) * 512] = f("gla_b_g")[gi]
    pv = np.zeros((128, PV_N), np.float32)
    for l in range(4):
        pv[:, PV_NW + l * 8:PV_NW + (l + 1) * 8] = _pcol(f("norm_w")[l])
    pv[:, PV_FNW:PV_FNW + 8] = _pcol(f("final_norm_w"))
    for j in range(2):
        pv[:, PV_GN + j * 8:PV_GN + (j + 1) * 8] = _pcol(f("gla_gn_w")[j])
        pv[:, PV_DWB + j * 8:PV_DWB + (j + 1) * 8] = _pcol(f("conv_dw_b")[j])
        pv[:, PV_LNW + j * 8:PV_LNW + (j + 1) * 8] = _pcol(f("conv_ln_w")[j])
        pv[:, PV_LNB + j * 8:PV_LNB + (j + 1) * 8] = _pcol(f("conv_ln_b")[j])
        dw = f("conv_dw_w")[j]
        for g in range(8):
            pv[:, PV_DW + (j * 8 + g) * 31:PV_DW + (j * 8 + g + 1) * 31] = dw[:, g * 128:(g + 1) * 128].T
    return {"wts": wts, "wg": wg, "wg2a": wg2a, "pv": pv}


_CACHE = {}


def run(inputs, seq=SEQ_FULL, nlayers=4, ncores=8, trace=False):
    x = np.asarray(inputs["x"], dtype=np.float32)
    wmap = prep_weights(inputs)
    key = (seq, nlayers)
    if key not in _CACHE:
        _CACHE[key] = build_program(seq, nlayers)[0]
    nc = _CACHE[key]
    in_maps = []
    for b in range(ncores):
        m = dict(wmap)
        m["xT"] = np.ascontiguousarray(x[b, :seq, :].T)
        in_maps.append(m)
    res = run_bass_kernel_spmd(nc, in_maps, core_ids=list(range(ncores)), trace=trace)
    out = np.stack([np.ascontiguousarray(r["outT"].T) for r in res.results], axis=0)
    return out.astype(np.float32), res


def kernel(**inputs):
    out, _ = run(inputs)
    return out
```

```python
import numpy as np
from contextlib import ExitStack
import concourse.bass as bass
import concourse.mybir as mybir
from concourse.bass_utils import run_bass_kernel_spmd

dt = mybir.dt
AF = mybir.ActivationFunctionType
ALU = mybir.AluOpType
F32, BF16 = dt.float32, dt.bfloat16

D = 1024
KC = 8
SEQ_FULL = 4096
T = 1024
NSUB = T // 512
NCH = T // 128
EPS = 1e-6
DKH = 128
NSLOT = 6
UNITS_PER_LAYER = 16
ARENA = 212736
ATOM = 64
COMPUTE = ("pe", "act", "dve", "pool")
STREAMS = ("pe", "act", "dve", "pool", "sp")

PV_NW, PV_FNW, PV_GN, PV_DWB, PV_LNW, PV_LNB, PV_DW, PV_N = 0, 32, 40, 56, 72, 88, 104, 600


class Op:
    __slots__ = ("id", "stream", "emit", "is_dma", "key", "ordinal", "deps",
                 "waits", "signal", "sigval", "seq", "vc")


class Sched:
    def __init__(self, nc, arena_bytes):
        self.nc = nc
        self.ops = []
        self.nat_sb = (arena_bytes + ATOM - 1) // ATOM
        self.nat_ps = 16384 // ATOM
        n = self.nat_sb + self.nat_ps
        self.W = np.full(n, -1, np.int64)
        self.R = {s: np.full(n, -1, np.int64) for s in COMPUTE}
        self.RD = np.full(n, -1, np.int64)
        self.dram = {}
        self.key_count = {}
        self.key_last = {}
        self.seq = {s: 0 for s in STREAMS}

    def rng(self, ap):
        sp = str(ap.space)
        es = mybir.dt.size(ap.dtype)
        dims = ap.ap
        pstride = dims[0][0]
        off = ap.offset % pstride if pstride > 0 else ap.offset
        ext = 1
        for (st, cnt) in dims[1:]:
            ext += (cnt - 1) * abs(st)
        lo = off * es
        hi = (off + ext) * es
        a0 = lo // ATOM
        a1 = (hi + ATOM - 1) // ATOM
        if "PSUM" in sp:
            a0 += self.nat_sb
            a1 += self.nat_sb
        return a0, a1

    def op(self, stream, emit, reads=(), writes=(), dreads=(), dwrites=(), dma_key=None):
        o = Op()
        o.id = len(self.ops)
        o.stream = stream
        o.emit = emit
        o.is_dma = dma_key is not None
        o.key = dma_key
        o.signal = False
        o.sigval = 0
        o.ordinal = 0
        o.waits = []
        o.seq = self.seq[stream]
        self.seq[stream] += 1
        deps = set()
        rr = [self.rng(a) for a in reads]
        wr = [self.rng(a) for a in writes]
        for (a0, a1) in rr:
            deps.update(np.unique(self.W[a0:a1]).tolist())
        for (a0, a1) in wr:
            deps.update(np.unique(self.W[a0:a1]).tolist())
            for s in COMPUTE:
                deps.update(np.unique(self.R[s][a0:a1]).tolist())
            deps.update(np.unique(self.RD[a0:a1]).tolist())
        for name in dreads:
            d = self.dram.setdefault(name, {"W": -1, "R": {}, "RD": -1})
            deps.add(d["W"])
        for name in dwrites:
            d = self.dram.setdefault(name, {"W": -1, "R": {}, "RD": -1})
            deps.add(d["W"])
            deps.update(d["R"].values())
            deps.add(d["RD"])
        if o.is_dma:
            deps.add(self.key_last.get(dma_key, -1))
            self.key_count[dma_key] = self.key_count.get(dma_key, 0) + 1
            o.ordinal = self.key_count[dma_key]
            self.key_last[dma_key] = o.id
            for (a0, a1) in rr:
                deps.update(np.unique(self.RD[a0:a1]).tolist())
        deps.discard(-1)
        for (a0, a1) in rr:
            if o.is_dma:
                self.RD[a0:a1] = o.id
            else:
                self.R[stream][a0:a1] = o.id
        for (a0, a1) in wr:
            self.W[a0:a1] = o.id
            for s in COMPUTE:
                self.R[s][a0:a1] = -1
            self.RD[a0:a1] = -1
        for name in dreads:
            d = self.dram[name]
            if o.is_dma:
                d["RD"] = o.id
            else:
                d["R"][stream] = o.id
        for name in dwrites:
            d = self.dram[name]
            d["W"] = o.id
            d["R"] = {}
            d["RD"] = -1
        o.deps = deps
        self.ops.append(o)
        return o

    def resolve(self):
        ops = self.ops
        vc = {s: {} for s in STREAMS}
        for o in ops:
            cur = vc[o.stream]
            need = {}
            for did in o.deps:
                d = ops[did]
                if d.is_dma:
                    sem = ("k", d.key)
                    val = d.ordinal
                else:
                    if d.stream == "pe" and o.stream == "pe":
                        continue
                    sem = ("s", d.stream)
                    val = d.seq + 1
                if val > need.get(sem, (0, None))[0]:
                    need[sem] = (val, d)
            for sem, (val, d) in need.items():
                if cur.get(sem, 0) >= val:
                    continue
                o.waits.append((sem, d))
                d.signal = True
                for k2, v2 in d.vc.items():
                    if cur.get(k2, 0) < v2:
                        cur[k2] = v2
                if cur.get(sem, 0) < val:
                    cur[sem] = val
            o.vc = dict(cur)
            if o.is_dma:
                k = ("k", o.key)
                o.vc[k] = max(o.vc.get(k, 0), o.ordinal)
            else:
                k = ("s", o.stream)
                o.vc[k] = max(o.vc.get(k, 0), o.seq + 1)
        cnt = {s: 0 for s in COMPUTE}
        for o in ops:
            if not o.is_dma and o.signal:
                cnt[o.stream] += 1
                o.sigval = cnt[o.stream]
        return cnt

    def emit_all(self, final_keys=()):
        nc = self.nc
        self.resolve()
        with ExitStack() as es:
            sems = {}
            for s in COMPUTE:
                sems[("s", s)] = es.enter_context(nc.semaphore("s_" + s))
            for i, k in enumerate(self.key_count):
                sems[("k", k)] = es.enter_context(nc.semaphore("k%d" % i))
            block = es.enter_context(nc.Block())
            by_stream = {s: [o for o in self.ops if o.stream == s] for s in STREAMS}

            def run(eng, stream):
                for o in by_stream[stream]:
                    for (sem, d) in o.waits:
                        if d.is_dma:
                            eng.wait_ge(sems[sem], 16 * d.ordinal)
                        else:
                            eng.wait_ge(sems[sem], d.sigval)
                    ins = o.emit(eng)
                    if o.is_dma:
                        ins.then_inc(sems[("k", o.key)], 16)
                    elif o.signal:
                        ins.then_inc(sems[("s", o.stream)], 1)
                if stream == "sp":
                    for k in final_keys:
                        eng.wait_ge(sems[("k", k)], 16 * self.key_count[k])

            @block.tensor
            def _(e):
                run(e, "pe")

            @block.scalar
            def _(e):
                run(e, "act")

            @block.vector
            def _(e):
                run(e, "dve")

            @block.gpsimd
            def _(e):
                run(e, "pool")

            @block.sync
            def _(e):
                run(e, "sp")


def build_program(seq=SEQ_FULL, nlayers=4):
    ntiles = seq // T
    nc = bass.Bass("TRN2", target_bir_lowering=False)
    xTd = nc.dram_tensor("xT", [D, seq], F32, kind="ExternalInput").ap()
    wts = nc.dram_tensor("wts", [4 * UNITS_PER_LAYER * 128, 2048], F32, kind="ExternalInput").ap()
    wgd = nc.dram_tensor("wg", [128, 2 * KC * 16], F32, kind="ExternalInput").ap()
    wg2d = nc.dram_tensor("wg2a", [17, 2 * 512], F32, kind="ExternalInput").ap()
    pvd = nc.dram_tensor("pv", [128, PV_N], F32, kind="ExternalInput").ap()
    outd = nc.dram_tensor("outT", [D, seq], F32, kind="ExternalOutput").ap()
    wbf = nc.dram_tensor("wbf", [4 * UNITS_PER_LAYER * 128, 2048], BF16, kind="Internal").ap()

    A = nc.alloc_sbuf_tensor("arena", [128, ARENA], dt.uint8)
    P = nc.alloc_psum_tensor("ps", [128, 4096], F32)
    S = Sched(nc, ARENA)
    off = [0]

    def sb(shape, d, at=None):
        nb = int(np.prod(shape[1:])) * mybir.dt.size(d)
        n = (nb + 63) // 64 * 64
        if at is None:
            o = off[0]
            off[0] += n
        else:
            o = at
        assert o + n <= ARENA, (o, n)
        v = A[:, o:o + nb].bitcast(d)
        if len(shape) == 3:
            v = v.rearrange("p (a b) -> p a b", a=shape[1])
        elif len(shape) == 4:
            v = v.rearrange("p (a b c) -> p a b c", a=shape[1], b=shape[2])
        return v

    xT = sb([128, KC, T], F32)
    hT = sb([128, KC, T], BF16)
    og = sb([128, KC, T], BF16)
    ring = [sb([128, KC, 256], BF16) for _ in range(NSLOT)]
    TMP = [sb([128, 512], F32) for _ in range(8)]
    TMPB = [sb([128, 512], BF16) for _ in range(4)]
    STAT = [sb([128, 512], F32) for _ in range(4)]
    ones_bf = sb([128, 128], BF16)
    ident = sb([128, 128], BF16)
    identB = sb([128, 32], BF16)
    utri = sb([128, 128], F32)
    mask = sb([128, 128], F32)
    ones_f = sb([128, 128], F32)
    cst = sb([128, 16], F32)
    pv = sb([128, PV_N], F32)
    dwh = sb([128, 496], F32)
    hln = sb([128, 32], F32)
    wg_bf = sb([128, 2 * KC * 16], BF16)
    wg2a = sb([128, 1024], F32)
    g_aug = sb([128, T], F32)
    Sst = sb([128, 8, 256], F32)
    Sall = sb([128, NCH, 256], BF16)
    stg = [sb([128, 1024], F32) for _ in range(2)]
    hist = sb([128, 16, 30], BF16)
    base = off[0]
    sp_ = sb([128, NCH, 512], F32)
    E1 = sb([128, T], F32)
    E2 = sb([128, T], F32)
    qdec = sb([128, T], BF16)
    kinv = sb([128, T], BF16)
    kendT = sb([128, T], BF16)
    kend = sb([128, NCH, 128], BF16)
    vtok = sb([128, NCH, 256], BF16)
    sz = sb([128, 2, T], BF16)
    oT = sb([128, 2, T], F32)
    osq = sb([128, 2, T], BF16)
    scm = sb([128, NCH, 128], BF16)
    gla_end = off[0]
    off[0] = base
    uc = sb([128, KC, T], F32)
    ubuf = [sb([128, 30 + T], BF16) for _ in range(2)]
    dgp = [sb([128, 31, 32], BF16) for _ in range(2)]
    acc1 = sb([128, T], F32)
    acc2 = sb([128, T], F32)
    conv_end = off[0]
    assert max(gla_end, conv_end) <= ARENA

    cnt = {"ps": 0, "tmp": 0, "tmpb": 0}

    def psum():
        i = cnt["ps"] % 7
        cnt["ps"] += 1
        return P[:, i * 512:(i + 1) * 512]

    ps_stat = P[:, 7 * 512:8 * 512]

    def tmp():
        i = cnt["tmp"] % len(TMP)
        cnt["tmp"] += 1
        return TMP[i], i

    def tmpb():
        i = cnt["tmpb"] % len(TMPB)
        cnt["tmpb"] += 1
        return TMPB[i]

    def isap(v):
        return not isinstance(v, (int, float)) and v is not None

    def mm(out, lhsT, rhs, start, stop):
        S.op("pe", lambda e: e.matmul(out, lhsT=lhsT, rhs=rhs, start=start, stop=stop),
             reads=[lhsT, rhs], writes=[out])

    def mmt(out, lhsT, rhs, start, stop, tp):
        S.op("pe", lambda e: e.matmul(out, lhsT=lhsT, rhs=rhs, start=start, stop=stop, tile_position=tp),
             reads=[lhsT, rhs], writes=[out])

    def tr(out, in_):
        S.op("pe", lambda e: e.transpose(out, in_, ident), reads=[in_, ident], writes=[out])

    def act(out, in_, func, scale=1.0, bias=0.0):
        rd = [in_] + [v for v in (scale, bias) if isap(v)]
        S.op("act", lambda e: e.activation(out=out, in_=in_, func=func, bias=bias, scale=scale),
             reads=rd, writes=[out])

    def tt(eng, out, in0, in1, op):
        S.op(eng, lambda e: e.tensor_tensor(out=out, in0=in0, in1=in1, op=op),
             reads=[in0, in1], writes=[out])

    def stt(out, in0, scalar, in1, op0, op1):
        rd = [in0, in1] + ([scalar] if isap(scalar) else [])
        S.op("dve", lambda e: e.scalar_tensor_tensor(out=out, in0=in0, scalar=scalar, in1=in1,
                                                     op0=op0, op1=op1),
             reads=rd, writes=[out])

    def ts(eng, out, in0, s1, s2, op0, op1=None):
        rd = [in0] + [v for v in (s1, s2) if isap(v)]
        if op1 is None:
            S.op(eng, lambda e: e.tensor_scalar(out=out, in0=in0, scalar1=s1, scalar2=None, op0=op0),
                 reads=rd, writes=[out])
        else:
            S.op(eng, lambda e: e.tensor_scalar(out=out, in0=in0, scalar1=s1, scalar2=s2,
                                                op0=op0, op1=op1),
                 reads=rd, writes=[out])

    def cp(eng, out, in_):
        if eng == "act":
            S.op("act", lambda e: e.activation(out=out, in_=in_, func=AF.Copy), reads=[in_], writes=[out])
        else:
            S.op(eng, lambda e: e.tensor_copy(out=out, in_=in_), reads=[in_], writes=[out])

    def memset(eng, ap, val):
        S.op(eng, lambda e: e.memset(ap, val), writes=[ap])

    def aselect(ap, cmp_op):
        S.op("pool", lambda e: e.affine_select(out=ap, in_=ap, pattern=[[1, 128]], compare_op=cmp_op,
                                               fill=0.0, base=0, channel_multiplier=-1),
             reads=[ap], writes=[ap])

    def dma(stream, out, in_, key, reads=(), writes=(), dreads=(), dwrites=()):
        S.op(stream, lambda e: e.dma_start(out=out, in_=in_), reads=reads, writes=writes,
             dreads=dreads, dwrites=dwrites, dma_key=key)

    eps_col = cst[:, 0:1]
    one_col = cst[:, 1:2]

    def pvc(col):
        return pv[:, col:col + 1]

    dma("pool", wg_bf, wgd, "wgc", writes=[wg_bf])
    dma("sp", pv, pvd, "pv", writes=[pv])
    memset("pool", wg2a, 0.0)
    dma("sp", wg2a[0:17, :], wg2d, "wg2", writes=[wg2a])
    memset("pool", ones_bf, 1.0)
    memset("pool", ones_f, 1.0)
    memset("dve", cst[:, 0:1], EPS)
    memset("dve", cst[:, 1:2], 1.0)
    memset("pool", ident, 1.0)
    aselect(ident, ALU.is_equal)
    tt("pool", identB, ident[:, 0:32], ident[:, 32:64], ALU.add)
    tt("pool", identB, identB, ident[:, 64:96], ALU.add)
    tt("pool", identB, identB, ident[:, 96:128], ALU.add)
    memset("pool", utri, -1.0 / 16.0)
    aselect(utri, ALU.is_ge)
    memset("pool", mask, 1.0)
    aselect(mask, ALU.is_ge)
    memset("dve", Sst, 0.0)
    memset("dve", hist, 0.0)
    memset("dve", g_aug, 1.0)
    ts("dve", dwh, pv[:, PV_DW:PV_DW + 496], 1.0, None, ALU.mult)
    ts("dve", hln, pv[:, PV_LNW:PV_LNW + 32], -1.0, None, ALU.mult)
    dwh4 = dwh.rearrange("p (a k) -> p a k", k=31)
    wg4 = wg_bf.rearrange("p (g c j) -> p g c j", g=2, c=KC)

    CONV_ORDER = [0, 1, 2, 8, 3, 4, 9, 5, 6, 10, 7, 11, 12, 13, 14, 15]
    GLA_ORDER = [1, 0, 2] + list(range(3, 16))
    units = [(ti, l, (GLA_ORDER[u] if l % 2 == 0 else CONV_ORDER[u])) for ti in range(ntiles) for l in range(nlayers)
             for u in range(UNITS_PER_LAYER)]
    rstate = {"next_load": 0, "next_use": 0}

    stg_i = [0]

    def ring_load():
        n = rstate["next_load"]
        if n >= len(units):
            return
        (ti, l, u) = units[n]
        slot = n % NSLOT
        row = (l * UNITS_PER_LAYER + u) * 128
        dst = ring[slot].rearrange("p a b -> p (a b)")
        if ti == 0:
            for half in range(2):
                k = stg_i[0] % 2
                stg_i[0] += 1
                dma("sp", stg[k], wts[row:row + 128, half * 1024:(half + 1) * 1024], ("stg", k),
                    writes=[stg[k]])
                cp("act" if half == 0 else "dve", dst[:, half * 1024:(half + 1) * 1024], stg[k])
        else:
            dma("sp", dst, wbf[row:row + 128, :], ("ring", slot), writes=[ring[slot]],
                dreads=[("wbf", l, u)])
        rstate["next_load"] += 1

    def ring_acquire(expect):
        n = rstate["next_use"]
        assert units[n][1:] == expect, (units[n], expect)
        return ring[n % NSLOT]

    def ring_acquire_n(expect, k):
        n = rstate["next_use"] + k
        assert units[n][1:] == expect, (units[n], expect)
        return ring[n % NSLOT]

    def ring_release():
        n = rstate["next_use"]
        (ti, l, u) = units[n]
        if ti == 0 and ntiles > 1:
            slot = n % NSLOT
            row = (l * UNITS_PER_LAYER + u) * 128
            dma("sp", wbf[row:row + 128, :], ring[slot].rearrange("p a b -> p (a b)"), ("wst", slot),
                reads=[ring[slot]], dwrites=[("wbf", l, u)])
        rstate["next_use"] += 1
        ring_load()

    for _ in range(NSLOT):
        ring_load()

    def ssl(s):
        return slice(s * 512, (s + 1) * 512)

    def rstd_from(ms, scale, lbuf, rbuf):
        act(lbuf, ms, AF.Ln, scale=scale, bias=eps_col)
        act(rbuf, lbuf, AF.Exp, scale=-0.5)
        return rbuf

    def sigmoid_multi(srcs, scales=None, biases=None):
        n = len(srcs)
        scales = scales or [-1.0] * n
        biases = biases or [0.0] * n
        outs = [tmp()[0] for _ in range(n)]
        for i in range(n):
            act(outs[i], srcs[i], AF.Exp, scale=scales[i], bias=biases[i])
        for i in range(n):
            act(outs[i], outs[i], AF.Ln, bias=one_col)
        for i in range(n):
            act(outs[i], outs[i], AF.Exp, scale=-1.0)
        return outs

    def rmsnorm_s(wcol0, out_fn, s, between=None):
        ms = ps_stat
        sqs = {}
        def square(c):
            sqs[c] = tmpb()
            if c % 2 == 0:
                act(sqs[c], xT[:, c, ssl(s)], AF.Square)
            else:
                tt("dve", sqs[c], xT[:, c, ssl(s)], xT[:, c, ssl(s)], ALU.mult)
        for c in range(min(2, KC)):
            square(c)
        for c in range(KC):
            if between is not None:
                between(c)
            mm(ms, ones_bf, sqs[c], c == 0, c == KC - 1)
            if c + 2 < KC:
                square(c + 2)
        r = rstd_from(ms, 1.0 / D, STAT[0], STAT[1])
        for c in range(KC):
            out_fn(c, s, r, pvc(wcol0 + c))

    def rmsnorm(wcol0, out_fn):
        for s in range(NSUB):
            rmsnorm_s(wcol0, out_fn, s)

    def proj_fm(U, col0, src, s, evac):
        ps = psum()
        for c in range(KC):
            mm(ps, U[:, c, col0:col0 + 128], src[:, c, ssl(s)], c == 0, c == KC - 1)
        evac(ps)

    def proj_ps(U, col0, src, s):
        ps = psum()
        for c in range(KC):
            mm(ps, U[:, c, col0:col0 + 128], src[:, c, ssl(s)], c == 0, c == KC - 1)
        return ps

    def outproj(l, nxt, during_s0=()):
        Us = []
        for j in range(4):
            Us.append(ring_acquire_n((l, 12 + j), j))

        def group(ob, s):
            j, m = ob // 2, ob % 2
            ps = proj_ps(Us[j], m * 128, og, s)
            tt("dve", xT[:, ob, ssl(s)], xT[:, ob, ssl(s)], ps, ALU.add)

        pend = list(during_s0)
        for ob in range(KC):
            group(ob, 0)
            if ob % 2 == 0 and pend:
                pend.pop(0)()
        for th in pend:
            th()

        def between(c):
            group(c, 1)
            if c % 2 == 1:
                ring_release()
        rmsnorm_s(nxt[0], nxt[1], 0, between=between)
        rmsnorm_s(nxt[0], nxt[1], 1)

    def hnorm_out(c, s, r, wc):
        stt(hT[:, c, ssl(s)], xT[:, c, ssl(s)], wc, r, ALU.mult, ALU.mult)

    def gla_layer(l, first, nxt):
        gi = l // 2
        if first:
            rmsnorm(PV_NW + l * 8, hnorm_out)
        for s in range(NSUB):
            gp = psum()[0:16, :]
            for c in range(KC):
                mm(gp, wg4[:, gi, c, :], hT[:, c, ssl(s)], c == 0, c == KC - 1)
            cp("act", g_aug[0:16, ssl(s)], gp)
        gla_vproj(l, 0)
        for j in range(NCH):
            xp = psum()
            mm(xp, g_aug[0:17, j * 128:(j + 1) * 128], wg2a[0:17, gi * 512:(gi + 1) * 512], True, True)
            e_, _ = tmp()
            act(e_, xp, AF.Exp, scale=-1.0)
            act(sp_[:, j, :], e_, AF.Ln, bias=one_col)
        for h in range(4):
            gla_head(l, gi, h)
        outproj(l, nxt)

    def gla_vproj(l, h):
        U = ring_acquire((l, 3 * h + 1))
        for jp in range(NCH // 2):
            vp = psum()
            for jj in range(2):
                j = jp * 2 + jj
                for c in range(KC):
                    mm(vp[:, jj * 256:(jj + 1) * 256], hT[:, c, j * 128:(j + 1) * 128], U[:, c, :],
                       c == 0, c == KC - 1)
            cp("act" if jp % 2 == 0 else "dve",
               vtok[:, jp * 2:jp * 2 + 2, :].rearrange("p a b -> p (a b)"), vp)
        ring_release()

    def gla_head(l, gi, h):
        Sh = Sst[:, gi * 4 + h, :]
        bt = [psum(), psum()]
        for j in range(NCH):
            mm(bt[j // 4][:, (j % 4) * 128:(j % 4 + 1) * 128], sp_[:, j, h * 128:(h + 1) * 128], utri,
               True, True)
        for s in range(NSUB):
            act(E1[:, ssl(s)], bt[s], AF.Exp)
            act(E2[:, ssl(s)], bt[s], AF.Exp, scale=-1.0)
        U = ring_acquire((l, 3 * h))
        for s in range(NSUB):
            def evq(ps, s=s):
                stt(qdec[:, ssl(s)], ps, DKH ** -0.5, E1[:, ssl(s)], ALU.mult, ALU.mult)
            proj_fm(U, 0, hT, s, evq)

            def evk(ps, s=s):
                tt("dve", kinv[:, ssl(s)], ps, E2[:, ssl(s)], ALU.mult)
                for jj in range(4):
                    j = s * 4 + jj
                    stt(kendT[:, j * 128:(j + 1) * 128], ps[:, jj * 128:(jj + 1) * 128],
                        E1[:, j * 128 + 127:j * 128 + 128], E2[:, j * 128:(j + 1) * 128],
                        ALU.mult, ALU.mult)
            proj_fm(U, 128, hT, s, evk)
        ring_release()
        if h != 0:
            gla_vproj(l, h)
        tp = psum().bitcast(BF16)
        for j in range(NCH):
            tr(tp[:, j * 128:(j + 1) * 128], kendT[:, j * 128:(j + 1) * 128])
        cp("act", kend.rearrange("p a b -> p (a b)"), tp)
        dsb = [psum() for _ in range(NCH // 2)]
        for j in range(NCH):
            mm(dsb[j // 2][:, (j % 2) * 256:(j % 2 + 1) * 256], kend[:, j, :], vtok[:, j, :], True, True)
        cp("dve", Sall[:, 0, :], Sh)
        for j in range(NCH):
            dcol = E1[:, j * 128 + 127:j * 128 + 128]
            dsj = dsb[j // 2][:, (j % 2) * 256:(j % 2 + 1) * 256]
            if j + 1 < NCH:
                stt(Sall[:, j + 1, :], Sh, dcol, dsj, ALU.mult, ALU.add)
            stt(Sh, Sh, dcol, dsj, ALU.mult, ALU.add)
        scb = [psum() for _ in range(NCH // 4)]
        for j in range(NCH):
            jsl = slice(j * 128, (j + 1) * 128)
            mm(scb[j // 4][:, (j % 4) * 128:(j % 4 + 1) * 128], kinv[:, jsl], qdec[:, jsl], True, True)
        for b in range(NCH // 4):
            tt("dve", scm[:, b * 4:(b + 1) * 4, :], scb[b].rearrange("p (a i) -> p a i", a=4),
               mask.unsqueeze(1).broadcast_to([128, 4, 128]), ALU.mult)
        U = ring_acquire((l, 3 * h + 2))
        for m in range(2):
            pss = [proj_ps(U, m * 128, hT, s) for s in range(NSUB)]
            gs = sigmoid_multi(pss)
            for s in range(NSUB):
                tt("dve", sz[:, m, ssl(s)], pss[s], gs[s], ALU.mult)
        ring_release()
        op_banks = []
        for jp in range(NCH // 2):
            opb = psum()
            op_banks.append(opb)
            for jj in range(2):
                j = jp * 2 + jj
                jsl = slice(j * 128, (j + 1) * 128)
                for m in range(2):
                    dst = opb[:, m * 256 + jj * 128:m * 256 + jj * 128 + 128]
                    mm(dst, vtok[:, j, m * 128:(m + 1) * 128], scm[:, j, :], True, False)
                    mm(dst, Sall[:, j, m * 128:(m + 1) * 128], qdec[:, jsl], False, True)
            pin = opb.rearrange("p (m i) -> p m i", m=2)
            cp("act", oT[:, :, jp * 256:(jp + 1) * 256], pin)
            tt("pool", osq[:, :, jp * 256:(jp + 1) * 256], oT[:, :, jp * 256:(jp + 1) * 256],
               oT[:, :, jp * 256:(jp + 1) * 256], ALU.mult)
        for s in range(NSUB):
            ms = psum()
            for m in range(2):
                mm(ms, ones_bf, osq[:, m, ssl(s)], m == 0, m == 1)
            r = rstd_from(ms, 1.0 / 256.0, STAT[0], STAT[1])
            for m in range(2):
                t1, _ = tmp()
                stt(t1, oT[:, m, ssl(s)], pvc(PV_GN + gi * 8 + h * 2 + m), r, ALU.mult, ALU.mult)
                tt("pool", og[:, h * 2 + m, ssl(s)], t1, sz[:, m, ssl(s)], ALU.mult)

    def conv_layer(l, first, nxt):
        ci = l // 2
        if first:
            rmsnorm(PV_NW + l * 8, hnorm_out)

        def conv_A(g):
            U = ring_acquire((l, g))
            ub = ubuf[g % 2]
            cp("pool", ub[:, 0:30], hist[:, ci * 8 + g, :])
            gps = [proj_ps(U, 128, hT, s) for s in range(NSUB)]
            gs = sigmoid_multi(gps)
            aps = [proj_ps(U, 0, hT, s) for s in range(NSUB)]
            for s in range(NSUB):
                tt("dve", ub[:, 30 + s * 512:30 + (s + 1) * 512], aps[s], gs[s], ALU.mult)
            ring_release()
            cp("pool", hist[:, ci * 8 + g, :], ub[:, T:T + 30])

        tapsets = [list(range(8 * j, min(8 * j + 8, 31))) for j in range(4)]

        def build_dg(g):
            idb = identB.unsqueeze(1).broadcast_to([128, 31, 32])
            wb = dwh4[:, ci * 8 + g, :].unsqueeze(2).broadcast_to([128, 31, 32])
            tt("dve", dgp[g % 2], idb, wb, ALU.mult)

        def conv_B(g):
            ub = ubuf[g % 2]
            dgg = dgp[g % 2]
            bcol = pvc(PV_DWB + ci * 8 + g)
            cpss = []
            for s in range(NSUB):
                bk = [psum() for _ in range(4)]
                for t in range(8):
                    for i in range(4):
                        for j in range(4):
                            if t >= len(tapsets[j]):
                                continue
                            k = tapsets[j][t]
                            mmt(bk[i][32 * j:32 * j + 32, :], dgg[32 * i:32 * i + 32, k, :],
                                ub[32 * i:32 * i + 32, s * 512 + k:s * 512 + k + 512],
                                t == 0, t == len(tapsets[j]) - 1, (32 * i, 32 * j))
                pis = []
                for i in range(4):
                    pi_ = tmpb()
                    cp("act" if i % 2 == 0 else "dve", pi_, bk[i])
                    pis.append(pi_)
                cps = psum()
                for i in range(4):
                    mmt(cps[32 * i:32 * i + 32, :], identB, pis[i], True, True, (0, 32 * i))
                cpss.append(cps)
            if g + 1 < KC:
                build_dg(g + 1)
            for s in range(NSUB):
                cps = cpss[s]
                act(uc[:, g, ssl(s)], cps, AF.Identity, bias=bcol)
                q_, _ = tmp()
                act(q_, cps, AF.Square, bias=bcol)
                if g == 0:
                    cp("pool", acc1[:, ssl(s)], uc[:, g, ssl(s)])
                    cp("pool", acc2[:, ssl(s)], q_)
                else:
                    tt("pool", acc1[:, ssl(s)], acc1[:, ssl(s)], uc[:, g, ssl(s)], ALU.add)
                    tt("pool", acc2[:, ssl(s)], acc2[:, ssl(s)], q_, ALU.add)

        def zblock(zb):
            zu, m = zb // 2, zb % 2
            U = ring_acquire((l, 8 + zu))
            pss = [proj_ps(U, m * 128, hT, s) for s in range(NSUB)]
            gs = sigmoid_multi(pss)
            for s in range(NSUB):
                tt("dve", og[:, zb, ssl(s)], pss[s], gs[s], ALU.mult)
            if m == 1:
                ring_release()

        build_dg(0)
        conv_A(0)
        for g in range(KC):
            if g + 1 < KC:
                conv_A(g + 1)
            conv_B(g)
            if g % 2 == 1:
                zblock(g - 1)
                zblock(g)

        mus, rs, mrs = [], [], []
        for s in range(NSUB):
            mp = psum()
            mm(mp, ones_f, acc1[:, ssl(s)], True, True)
            qp = psum()
            mm(qp, ones_f, acc2[:, ssl(s)], True, True)
            mu, r, mr = acc1[:, ssl(s)], acc2[:, ssl(s)], STAT[s]
            musq, var = STAT[2], STAT[3]
            ts("dve", mu, mp, 1.0 / D, None, ALU.mult)
            tt("dve", musq, mu, mu, ALU.mult)
            stt(var, qp, 1.0 / D, musq, ALU.mult, ALU.subtract)
            rstd_from(var, 1.0, musq, r)
            tt("dve", mr, mu, r, ALU.mult)
            mus.append(mu)
            rs.append(r)
            mrs.append(mr)

        def ln_front(s):
            r, mr = rs[s], mrs[s]
            for g in range(KC):
                u_ = uc[:, g, ssl(s)]
                stt(u_, u_, pvc(PV_LNW + ci * 8 + g), r, ALU.mult, ALU.mult)
                stt(u_, mr, hln[:, ci * 8 + g:ci * 8 + g + 1], u_, ALU.mult, ALU.add)

        def ln_back(s, blocks):
            gs = sigmoid_multi([uc[:, g, ssl(s)] for g in blocks], scales=[-1.0] * len(blocks),
                               biases=[hln[:, 16 + ci * 8 + g:16 + ci * 8 + g + 1] for g in blocks])
            for i, g in enumerate(blocks):
                u_ = uc[:, g, ssl(s)]
                stt(u_, u_, pvc(PV_LNB + ci * 8 + g), gs[i], ALU.add, ALU.mult)
                tt("pool", og[:, g, ssl(s)], u_, og[:, g, ssl(s)], ALU.mult)

        ln_front(0)
        ln_front(1)
        ln_back(0, list(range(KC)))
        outproj(l, nxt, during_s0=[lambda: ln_back(1, [0, 1, 2, 3]), lambda: ln_back(1, [4, 5, 6, 7])])

    store_keys = []
    for ti in range(ntiles):
        t0 = ti * T
        for c in range(KC):
            dma("sp", xT[:, c, :], xTd[c * 128:(c + 1) * 128, t0:t0 + T], ("x", c), writes=[xT[:, c, :]])
        def fin_out(c, s, r, wc, t0=t0):
            o_, i = tmp()
            stt(o_, xT[:, c, ssl(s)], wc, r, ALU.mult, ALU.mult)
            key = ("st", i)
            if key not in store_keys:
                store_keys.append(key)
            dma("sp", outd[c * 128:(c + 1) * 128, t0 + s * 512:t0 + (s + 1) * 512], o_, key, reads=[o_])

        for l in range(nlayers):
            if l + 1 < nlayers:
                nxt = (PV_NW + (l + 1) * 8, hnorm_out)
            else:
                nxt = (PV_FNW, fin_out)
            if l % 2 == 0:
                gla_layer(l, l == 0, nxt)
            else:
                conv_layer(l, l == 0, nxt)

    S.emit_all(final_keys=store_keys)
    return nc, len(S.ops)


def _unit(wcols):
    return np.ascontiguousarray(wcols.reshape(KC, 128, 256).transpose(1, 0, 2).reshape(128, KC * 256))


def _pcol(v):
    return np.ascontiguousarray(np.asarray(v).reshape(KC, 128).T)


def prep_weights(inp):
    f = lambda k: np.asarray(inp[k], dtype=np.float32)
    gwi, gwo = f("gla_w_in"), f("gla_w_out")
    cwi, cwo = f("conv_w_in"), f("conv_w_out")
    units = []
    for l in range(4):
        j = l // 2
        if l % 2 == 0:
            W = gwi[j]
            for h in range(4):
                units.append(_unit(np.concatenate([W[:, h * 128:(h + 1) * 128],
                                                   W[:, 512 + h * 128:512 + (h + 1) * 128]], axis=1)))
                units.append(_unit(W[:, 1024 + h * 256:1024 + (h + 1) * 256]))
                units.append(_unit(W[:, 2048 + h * 256:2048 + (h + 1) * 256]))
            for o in range(4):
                units.append(_unit(gwo[j][:, o * 256:(o + 1) * 256]))
        else:
            W = cwi[j]
            for g in range(8):
                units.append(_unit(np.concatenate([W[:, g * 128:(g + 1) * 128],
                                                   W[:, 1024 + g * 128:1024 + (g + 1) * 128]], axis=1)))
            for o in range(4):
                units.append(_unit(W[:, 2048 + o * 256:2048 + (o + 1) * 256]))
            for o in range(4):
                units.append(_unit(cwo[j][:, o * 256:(o + 1) * 256]))
    wts = np.ascontiguousarray(np.concatenate(units, axis=0))
    wg = np.stack([gwi[gi][:, 3072:3088].reshape(KC, 128, 16).transpose(1, 0, 2) for gi in range(2)], axis=1)
    wg = np.ascontiguousarray(wg.reshape(128, 2 * KC * 16))
    wg2a = np.zeros((17, 1024), np.float32)
    for gi in range(2):
        wg2a[0:16, gi * 512:(gi + 1) * 512] = f("gla_w_g2")[gi]
        wg2a[16, gi * 512:(gi + 1) * 512] = f("gla_b_g")[gi]
    pv = np.zeros((128, PV_N), np.float32)
    for l in range(4):
        pv[:, PV_NW + l * 8:PV_NW + (l + 1) * 8] = _pcol(f("norm_w")[l])
    pv[:, PV_FNW:PV_FNW + 8] = _pcol(f("final_norm_w"))
    for j in range(2):
        pv[:, PV_GN + j * 8:PV_GN + (j + 1) * 8] = _pcol(f("gla_gn_w")[j])
        pv[:, PV_DWB + j * 8:PV_DWB + (j + 1) * 8] = _pcol(f("conv_dw_b")[j])
        pv[:, PV_LNW + j * 8:PV_LNW + (j + 1) * 8] = _pcol(f("conv_ln_w")[j])
        pv[:, PV_LNB + j * 8:PV_LNB + (j + 1) * 8] = _pcol(f("conv_ln_b")[j])
        dw = f("conv_dw_w")[j]
        for g in range(8):
            pv[:, PV_DW + (j * 8 + g) * 31:PV_DW + (j * 8 + g + 1) * 31] = dw[:, g * 128:(g + 1) * 128].T
    return {"wts": wts, "wg": wg, "wg2a": wg2a, "pv": pv}


_CACHE = {}


def run(inputs, seq=SEQ_FULL, nlayers=4, ncores=8, trace=False):
    x = np.asarray(inputs["x"], dtype=np.float32)
    wmap = prep_weights(inputs)
    key = (seq, nlayers)
    if key not in _CACHE:
        _CACHE[key] = build_program(seq, nlayers)[0]
    nc = _CACHE[key]
    in_maps = []
    for b in range(ncores):
        m = dict(wmap)
        m["xT"] = np.ascontiguousarray(x[b, :seq, :].T)
        in_maps.append(m)
    res = run_bass_kernel_spmd(nc, in_maps, core_ids=list(range(ncores)), trace=trace)
    out = np.stack([np.ascontiguousarray(r["outT"].T) for r in res.results], axis=0)
    return out.astype(np.float32), res


def kernel(**inputs):
    out, _ = run(inputs)
    return out
```
